# Optimizing a Trainium2 kernel written in Bass

```python
import math
import jax, jax.numpy as jnp
from jax import lax
import numpy as np

D_MODEL = 4096
BATCH = 4
SEQ = 4096
DEPTH = 2

A_HEAD_DIM = 128
A_HEADS = D_MODEL // 2 // A_HEAD_DIM
A_KV_HEADS = 4
IDX_HEADS = 16
IDX_DIM = 64
DSA_TOPK = 256
RET_KEY_DIM = 256
RET_VAL_DIM = 256
RET_HEADS = D_MODEL // 2 // RET_VAL_DIM
RET_CHUNK = 128
RET_THETA = 10000.0
MLA_V = 128
MLA_HEADS = D_MODEL // MLA_V
MLA_Q_RANK = 1024
MLA_KV_RANK = 512
MLA_NOPE = 128
MLA_ROPE = 64
FFN_DIM = 14336
N_EXPERTS = 8
TOP_K_EXPERTS = 2
EXPERT_DIM = 5120
ROPE_THETA = 500000.0
Q_BLOCK = 128
LN_EPS = 1e-5
RMS_EPS = 1e-6
DEEPNORM_ALPHA = (2.0 * DEPTH) ** 0.25
DEEPNORM_BETA = (8.0 * DEPTH) ** -0.25

L0_COLS = (
    A_HEADS * A_HEAD_DIM,
    A_KV_HEADS * A_HEAD_DIM,
    A_KV_HEADS * A_HEAD_DIM,
    IDX_HEADS * IDX_DIM,
    IDX_DIM,
    IDX_HEADS,
    RET_HEADS * RET_KEY_DIM,
    RET_HEADS * RET_KEY_DIM,
    RET_HEADS * RET_VAL_DIM,
    RET_HEADS * RET_VAL_DIM,
)
L0_IN = sum(L0_COLS)
L0_MIX = A_HEADS * A_HEAD_DIM + RET_HEADS * RET_VAL_DIM
L1_DOWN = MLA_Q_RANK + MLA_KV_RANK + MLA_ROPE

kernel_name = "hybrid_dsa_retention_mla_moe_block"

F32 = jnp.float32


def layer_norm(x, g, b):
    xf = x.astype(F32)
    mu = jnp.mean(xf, -1, keepdims=True)
    var = jnp.mean(jnp.square(xf - mu), -1, keepdims=True)
    return ((xf - mu) * lax.rsqrt(var + LN_EPS) * g + b).astype(x.dtype)


def rms_norm(x, g):
    xf = x.astype(F32)
    return (xf * lax.rsqrt(jnp.mean(xf * xf, -1, keepdims=True) + RMS_EPS) * g).astype(x.dtype)


def rope_tables(T, rot_dim, theta):
    inv = theta ** (-jnp.arange(0, rot_dim, 2, dtype=F32) / rot_dim)
    ang = jnp.arange(T, dtype=F32)[:, None] * inv[None, :]
    return jnp.cos(ang), jnp.sin(ang)


def retention_tables(T):
    inv = 1.0 / (RET_THETA ** jnp.linspace(0.0, 1.0, RET_KEY_DIM // 2, dtype=F32))
    ang = jnp.arange(T, dtype=F32)[:, None] * inv[None, :]
    return jnp.cos(ang), jnp.sin(ang)


def apply_rotary(x, cos, sin):
    half = x.shape[-1] // 2
    x1 = x[..., :half].astype(F32)
    x2 = x[..., half:].astype(F32)
    c = cos[None, :, None, :]
    s = sin[None, :, None, :]
    return jnp.concatenate([x1 * c - x2 * s, x2 * c + x1 * s], -1).astype(x.dtype)


def partial_rotary(x, cos, sin):
    r = 2 * cos.shape[-1]
    return jnp.concatenate([apply_rotary(x[..., :r], cos, sin), x[..., r:]], -1)


def split_cols(a, sizes):
    offs, o = [], 0
    for s in sizes[:-1]:
        o += s
        offs.append(o)
    return jnp.split(a, offs, axis=-1)


def to_blocks(a):
    B, T = a.shape[:2]
    return jnp.swapaxes(a.reshape((B, T // Q_BLOCK, Q_BLOCK) + a.shape[2:]), 0, 1)


def from_blocks(a):
    a = jnp.swapaxes(a, 0, 1)
    return a.reshape((a.shape[0], a.shape[1] * a.shape[2]) + a.shape[3:])


def dsa_attention(q, k, v, qi, ki, wi):
    B, T = q.shape[:2]
    topk = min(DSA_TOPK, T // 4)
    n_rep = A_HEADS // A_KV_HEADS
    key_pos = jnp.arange(T)

    def block(args):
        qb, qib, wib, blk = args
        q_pos = blk * Q_BLOCK + jnp.arange(Q_BLOCK)
        causal = key_pos[None, :] <= q_pos[:, None]
        logits = jnp.einsum('bqhd,bsd->bqhs', qib, ki, preferred_element_type=F32) * IDX_DIM ** -0.5
        index_score = jnp.einsum('bqh,bqhs->bqs', wib.astype(F32) * IDX_HEADS ** -0.5, jax.nn.relu(logits))
        index_score = jnp.where(causal[None], index_score, -jnp.inf)
        _, sel = lax.top_k(index_score, topk)
        valid = sel <= q_pos[None, :, None]
        k_sel = jax.vmap(lambda kk, ii: kk[ii])(k, sel)
        v_sel = jax.vmap(lambda vv, ii: vv[ii])(v, sel)
        qg = qb.reshape(B, Q_BLOCK, A_KV_HEADS, n_rep, A_HEAD_DIM)
        s = jnp.einsum('bqgrd,bqkgd->bqgrk', qg, k_sel, preferred_element_type=F32) * A_HEAD_DIM ** -0.5
        s = jnp.where(valid[:, :, None, None, :], s, -jnp.inf)
        p = jax.nn.softmax(s, axis=-1).astype(v.dtype)
        o = jnp.einsum('bqgrk,bqkgd->bqgrd', p, v_sel)
        return o.reshape(B, Q_BLOCK, A_HEADS * A_HEAD_DIM)

    nb = T // Q_BLOCK
    out = lax.map(block, (to_blocks(q), to_blocks(qi), to_blocks(wi), jnp.arange(nb)))
    return from_blocks(out)


def retention_chunkwise(q, k, v, log_gamma):
    B, T, H, DK = q.shape
    DV = v.shape[-1]
    C = RET_CHUNK
    pos = jnp.arange(C, dtype=F32)
    diff = pos[:, None] - pos[None, :]
    decay_in = jnp.exp(jnp.where(diff[None] >= 0, log_gamma[:, None, None] * diff[None], -jnp.inf))
    q_decay = jnp.exp(log_gamma[:, None] * (pos[None] + 1.0))
    k_decay = jnp.exp(log_gamma[:, None] * (C - 1.0 - pos[None]))
    chunk_decay = jnp.exp(log_gamma * C)

    def chunks(a):
        return jnp.moveaxis(a.astype(F32).reshape(B, T // C, C, H, a.shape[-1]), (1, 3), (0, 2))

    def step(state, inp):
        qc, kc, vc = inp
        inner = jnp.einsum('bhid,bhjd->bhij', qc, kc) * decay_in[None]
        o = (jnp.einsum('bhij,bhjv->bhiv', inner, vc)
             + jnp.einsum('bhid,bhdv->bhiv', qc * q_decay[None, :, :, None], state))
        state = (state * chunk_decay[None, :, None, None]
                 + jnp.einsum('bhjd,bhjv->bhdv', kc * k_decay[None, :, :, None], vc))
        return state, o

    state0 = jnp.zeros((B, H, DK, DV), F32)
    _, o = lax.scan(step, state0, (chunks(q), chunks(k), chunks(v)))
    return jnp.moveaxis(o, (0, 2), (1, 3)).reshape(B, T, H, DV)


def retention_mixer(q, k, v, g, gn_g, cos, sin):
    B, T = q.shape[:2]
    log_gamma = jnp.log(1.0 - 2.0 ** (-5.0 - jnp.arange(RET_HEADS, dtype=F32)))
    q = apply_rotary(q, cos, sin)
    k = apply_rotary(k, cos, sin) * RET_KEY_DIM ** -0.5
    o = retention_chunkwise(q, k, v, log_gamma)
    mu = jnp.mean(o, -1, keepdims=True)
    var = jnp.mean(jnp.square(o - mu), -1, keepdims=True)
    o = ((o - mu) * lax.rsqrt(var + LN_EPS)).reshape(B, T, RET_HEADS * RET_VAL_DIM) * gn_g
    return (jax.nn.silu(g.astype(F32)) * o).astype(g.dtype)


def mla_attention(x, w_dq_dkv, q_norm_g, w_uq, kv_norm_g, w_ukv, cos, sin):
    B, T, _ = x.shape
    cq, ckv, kr = split_cols(x @ w_dq_dkv, (MLA_Q_RANK, MLA_KV_RANK, MLA_ROPE))
    q = (rms_norm(cq, q_norm_g) @ w_uq).reshape(B, T, MLA_HEADS, MLA_NOPE + MLA_ROPE)
    q_nope = q[..., :MLA_NOPE]
    q_rope = apply_rotary(q[..., MLA_NOPE:], cos, sin)
    k_rope = apply_rotary(kr[:, :, None, :], cos, sin)[:, :, 0]
    kv = (rms_norm(ckv, kv_norm_g) @ w_ukv).reshape(B, T, MLA_HEADS, MLA_NOPE + MLA_V)
    k_nope = kv[..., :MLA_NOPE]
    v = kv[..., MLA_NOPE:]
    scale = (MLA_NOPE + MLA_ROPE) ** -0.5
    key_pos = jnp.arange(T)

    def block(args):
        qn, qr, blk = args
        q_pos = blk * Q_BLOCK + jnp.arange(Q_BLOCK)
        s = (jnp.einsum('bqhd,bshd->bhqs', qn, k_nope, preferred_element_type=F32)
             + jnp.einsum('bqhd,bsd->bhqs', qr, k_rope, preferred_element_type=F32)) * scale
        s = jnp.where((key_pos[None, :] <= q_pos[:, None])[None, None], s, -jnp.inf)
        p = jax.nn.softmax(s, axis=-1).astype(v.dtype)
        return jnp.einsum('bhqs,bshd->bqhd', p, v).reshape(B, Q_BLOCK, MLA_HEADS * MLA_V)

    out = lax.map(block, (to_blocks(q_nope), to_blocks(q_rope), jnp.arange(T // Q_BLOCK)))
    return from_blocks(out)


def swiglu(x, w1, w3, w2):
    return (jax.nn.silu(x @ w1) * (x @ w3)) @ w2


def moe_swiglu(x, router, w1, w3, w2):
    B, T, D = x.shape
    xt = x.reshape(B * T, D)
    logits = jnp.dot(xt, router, preferred_element_type=F32)
    top_val, top_idx = lax.top_k(logits, TOP_K_EXPERTS)
    gates = jax.nn.softmax(top_val, axis=-1)
    flat_e = top_idx.reshape(-1)
    order = jnp.argsort(flat_e)
    tok = order // TOP_K_EXPERTS
    group_sizes = jnp.bincount(flat_e, length=N_EXPERTS).astype(jnp.int32)
    xs = xt[tok]
    h = jax.nn.silu(lax.ragged_dot(xs, w1, group_sizes)) * lax.ragged_dot(xs, w3, group_sizes)
    ys = lax.ragged_dot(h, w2, group_sizes) * gates.reshape(-1)[order][:, None].astype(h.dtype)
    y = jnp.zeros_like(xt).at[tok].add(ys.astype(xt.dtype))
    return y.reshape(B, T, D)


def even_layer(x, w_in, ret_gn_g, w_out, ln1_g, ln1_b, w1, w3, w2, ln2_g, ln2_b, rope_a, rope_i, rope_r):
    B, T, _ = x.shape
    qa, ka, va, qi, ki, wi, qb, kb, vb, gb = split_cols(x @ w_in, L0_COLS)
    qa = partial_rotary(qa.reshape(B, T, A_HEADS, A_HEAD_DIM), *rope_a)
    ka = partial_rotary(ka.reshape(B, T, A_KV_HEADS, A_HEAD_DIM), *rope_a)
    va = va.reshape(B, T, A_KV_HEADS, A_HEAD_DIM)
    qi = partial_rotary(qi.reshape(B, T, IDX_HEADS, IDX_DIM), *rope_i)
    ki = partial_rotary(ki.reshape(B, T, 1, IDX_DIM), *rope_i)[:, :, 0]
    ya = dsa_attention(qa, ka, va, qi, ki, wi)
    yb = retention_mixer(qb.reshape(B, T, RET_HEADS, RET_KEY_DIM),
                         kb.reshape(B, T, RET_HEADS, RET_KEY_DIM),
                         vb.reshape(B, T, RET_HEADS, RET_VAL_DIM),
                         gb, ret_gn_g, *rope_r)
    y = jnp.concatenate([ya, yb.astype(ya.dtype)], axis=-1) @ w_out
    x = layer_norm(DEEPNORM_ALPHA * x + y, ln1_g, ln1_b)
    return layer_norm(DEEPNORM_ALPHA * x + swiglu(x, w1, w3, w2), ln2_g, ln2_b)


def odd_layer(x, w_dq_dkv, q_norm_g, w_uq, kv_norm_g, w_ukv, w_out, ln1_g, ln1_b,
              router, we1, we3, we2, ln2_g, ln2_b, rope_c):
    y = mla_attention(x, w_dq_dkv, q_norm_g, w_uq, kv_norm_g, w_ukv, *rope_c) @ w_out
    x = layer_norm(DEEPNORM_ALPHA * x + y, ln1_g, ln1_b)
    return layer_norm(DEEPNORM_ALPHA * x + moe_swiglu(x, router, we1, we3, we2), ln2_g, ln2_b)


def setup_inputs(seed: int = 0) -> dict:
    key = jax.random.key(seed)
    ks = jax.random.split(key, 25)

    def w(k, shape, fan_in, scale=1.0):
        return jax.random.normal(k, shape, F32) * (scale * fan_in ** -0.5)

    def gain(k, n):
        return 1.0 + 0.02 * jax.random.normal(k, (n,), F32)

    def bias(k, n):
        return 0.02 * jax.random.normal(k, (n,), F32)

    D = D_MODEL
    b = DEEPNORM_BETA
    return {
        "x": jax.random.normal(ks[0], (BATCH, SEQ, D), F32),
        "l0_w_in": w(ks[1], (D, L0_IN), D),
        "l0_ret_gn_g": gain(ks[2], RET_HEADS * RET_VAL_DIM),
        "l0_w_out": w(ks[3], (L0_MIX, D), L0_MIX, b),
        "l0_ln1_g": gain(ks[4], D),
        "l0_ln1_b": bias(ks[5], D),
        "l0_ffn_w1": w(ks[6], (D, FFN_DIM), D),
        "l0_ffn_w3": w(ks[7], (D, FFN_DIM), D),
        "l0_ffn_w2": w(ks[8], (FFN_DIM, D), FFN_DIM, b),
        "l0_ln2_g": gain(ks[9], D),
        "l0_ln2_b": bias(ks[10], D),
        "l1_w_dq_dkv": w(ks[11], (D, L1_DOWN), D),
        "l1_q_norm_g": gain(ks[12], MLA_Q_RANK),
        "l1_w_uq": w(ks[13], (MLA_Q_RANK, MLA_HEADS * (MLA_NOPE + MLA_ROPE)), MLA_Q_RANK),
        "l1_kv_norm_g": gain(ks[14], MLA_KV_RANK),
        "l1_w_ukv": w(ks[15], (MLA_KV_RANK, MLA_HEADS * (MLA_NOPE + MLA_V)), MLA_KV_RANK),
        "l1_w_out": w(ks[16], (MLA_HEADS * MLA_V, D), MLA_HEADS * MLA_V, b),
        "l1_ln1_g": gain(ks[17], D),
        "l1_ln1_b": bias(ks[18], D),
        "l1_router": w(ks[19], (D, N_EXPERTS), D),
        "l1_moe_w1": w(ks[20], (N_EXPERTS, D, EXPERT_DIM), D),
        "l1_moe_w3": w(ks[21], (N_EXPERTS, D, EXPERT_DIM), D),
        "l1_moe_w2": w(ks[22], (N_EXPERTS, EXPERT_DIM, D), EXPERT_DIM, b),
        "l1_ln2_g": gain(ks[23], D),
        "l1_ln2_b": bias(ks[24], D),
    }


def reference(x, l0_w_in, l0_ret_gn_g, l0_w_out, l0_ln1_g, l0_ln1_b, l0_ffn_w1, l0_ffn_w3, l0_ffn_w2,
              l0_ln2_g, l0_ln2_b, l1_w_dq_dkv, l1_q_norm_g, l1_w_uq, l1_kv_norm_g, l1_w_ukv, l1_w_out,
              l1_ln1_g, l1_ln1_b, l1_router, l1_moe_w1, l1_moe_w3, l1_moe_w2, l1_ln2_g, l1_ln2_b):
    T = x.shape[1]
    rope_a = rope_tables(T, A_HEAD_DIM // 4, ROPE_THETA)
    rope_i = rope_tables(T, IDX_DIM // 4, ROPE_THETA)
    rope_r = retention_tables(T)
    rope_c = rope_tables(T, MLA_ROPE, ROPE_THETA)
    layer_params = (
        (l0_w_in, l0_ret_gn_g, l0_w_out, l0_ln1_g, l0_ln1_b, l0_ffn_w1, l0_ffn_w3, l0_ffn_w2, l0_ln2_g, l0_ln2_b),
        (l1_w_dq_dkv, l1_q_norm_g, l1_w_uq, l1_kv_norm_g, l1_w_ukv, l1_w_out, l1_ln1_g, l1_ln1_b,
         l1_router, l1_moe_w1, l1_moe_w3, l1_moe_w2, l1_ln2_g, l1_ln2_b),
    )
    for layer in range(DEPTH):
        p = layer_params[layer]
        if layer % 2 == 0:
            x = even_layer(x, *p, rope_a, rope_i, rope_r)
        else:
            x = odd_layer(x, *p, rope_c)
    return x
```

```python
from contextlib import ExitStack

import numpy as np
import concourse.bass as bass
import concourse.mybir as mybir
from concourse.bass_utils import run_bass_kernel_spmd

F32 = mybir.dt.float32
BF16 = mybir.dt.bfloat16
AF = mybir.ActivationFunctionType
ALU = mybir.AluOpType
AX = mybir.AxisListType

NCORES = 8
D = 4096
T = 4096
NT = 2048
NB = NT // 128
KC = D // 128
FFN = 14336
ALPHA = (2.0 * 2) ** 0.25
LN_EPS = 1e-5
RMS_EPS = 1e-6
NEG = -1.0e30
C_QA, C_KA, C_VA, C_QI, C_KI, C_WI, C_QB, C_KB, C_VB, C_GB, C_END = (
    0, 2048, 2560, 3072, 4096, 4160, 4176, 6224, 8272, 10320, 12368)


class Prog:
    def __init__(self, nc, stack):
        self.nc = nc
        self.eng = {"pe": nc.tensor, "act": nc.scalar, "dve": nc.vector, "pool": nc.gpsimd, "sp": nc.sync}
        self.sem = {k: stack.enter_context(nc.semaphore("sem_" + k)) for k in self.eng}
        self.cc = stack.enter_context(nc.semaphore("sem_cc"))
        self.ncc = 0
        self.cnt = {k: 0 for k in self.eng}
        self.last = None
        self.n = 0

    def _wait(self, e):
        if self.last is not None:
            sem, c = self.last
            self.eng[e].wait_ge(sem, c)

    def op(self, e, fn, chain=False, inc=1):
        if not chain:
            self._wait(e)
        ins = fn(self.eng[e])
        self.cnt[e] += inc
        ins.then_inc(self.sem[e], inc)
        self.last = (self.sem[e], self.cnt[e])
        self.n += 1
        return ins

    def dma(self, out, in_, e="sp"):
        return self.op(e, lambda g: g.dma_start(out=out, in_=in_), inc=16)

    def mm(self, out, lhsT, rhs, start, stop, chain=None):
        return self.op("pe", lambda g: g.matmul(out, lhsT, rhs, start=start, stop=stop),
                       chain=(not start) if chain is None else chain)

    def allgather(self, out, in_):
        self._wait("pool")
        ins = self.nc.gpsimd.collective_compute("AllGather", ALU.bypass, replica_groups=[list(range(NCORES))],
                                                ins=[in_.opt()], outs=[out.opt()])
        self.ncc += 1
        ins.then_inc(self.cc)
        self.last = (self.cc, self.ncc)
        self.n += 1

    def join_cc(self):
        for e in self.eng:
            self.eng[e].wait_ge(self.cc, self.ncc)

    def stream(self, stack, name):
        return [stack.enter_context(self.nc.semaphore(name)), 0]

    def dma_async(self, strm, out, in_, first=True):
        if first:
            self._wait("sp")
        ins = self.eng["sp"].dma_start(out=out, in_=in_)
        strm[1] += 16
        ins.then_inc(strm[0], 16)
        self.n += 1

    def wait_stream(self, e, strm):
        self.eng[e].wait_ge(strm[0], strm[1])

    def finish(self):
        for e in self.eng:
            self._wait(e)


def build_program(upto="all", debug=()):
    nc = bass.Bass("TRN2", target_bir_lowering=False, num_devices=NCORES)
    ext = {}
    _ctr = [0]

    def U(name):
        _ctr[0] += 1
        return "%s_%d" % (name, _ctr[0])

    def inp(name, shape, dt=F32):
        ext[name] = nc.dram_tensor(name, list(shape), dt, kind="ExternalInput").ap()
        return ext[name]

    def scratch(name, shape, dt):
        kind = "ExternalOutput" if name in debug else "Internal"
        return nc.dram_tensor(name, list(shape), dt, kind=kind).ap()

    xp = inp("xp", [T, D])
    out = nc.dram_tensor("out", [NT, D], F32, kind="ExternalOutput").ap()
    WSH = {"w_in": (D, C_END), "w_out0": (D, D), "f_w1": (D, FFN), "f_w3": (D, FFN), "f_w2": (FFN, D)}
    WSH.update({"w_dq": (D, 1600), "w_uq": (1024, 6144), "w_ukv": (512, 8192), "w_out1": (D, D),
                "m_w1a": (8 * D, 2560), "m_w1b": (8 * D, 2560), "m_w3a": (8 * D, 2560), "m_w3b": (8 * D, 2560),
                "m_w2a": (8 * 2560, D), "m_w2b": (8 * 2560, D)})
    if upto in ("proj", "post", "index", "dsa", "ret"):
        WSH = {"w_in": (D, C_END)}
    elif upto == "l0":
        WSH = {k: WSH[k] for k in ("w_in", "w_out0", "f_w1", "f_w3", "f_w2")}
    wsh_in = {k: inp(k, [r // NCORES, c]) for k, (r, c) in WSH.items()}
    gn_g = inp("gn_g", [1, 2048])
    ln_p = {k: inp(k, [1, D]) for k in ("l0_ln1_g", "l0_ln1_b", "l0_ln2_g", "l0_ln2_b",
                                        "l1_ln1_g", "l1_ln1_b", "l1_ln2_g", "l1_ln2_b")}
    qn_g = inp("qn_g", [1, 1024]); kvn_g = inp("kvn_g", [1, 512])
    routerT = inp("routerT", [8, D])
    cosC = inp("cosC", [T, 32]); sinC = inp("sinC", [T, 32])
    widx_in = inp("widx", [128, 16], mybir.dt.int32); ridx_in = inp("ridx", [128, 16], mybir.dt.int32)
    cosA = inp("cosA", [T, 256]); sinA = inp("sinA", [T, 256])
    cosI = inp("cosI", [T, 128]); sinI = inp("sinI", [T, 128])
    cosR = inp("cosR", [T, 128]); sinR = inp("sinR", [T, 128])
    triT_in = inp("triT", [128, 128])
    negtri_in = inp("negtri", [128, 128])
    cc_in = inp("cc", [128, 4])
    rdecT_in = inp("rdecT", [128, 8 * 128])
    rvec_in = inp("rvec", [128, 32])

    with ExitStack() as st:
        P = Prog(nc, st)

        wb = {}
        with ExitStack() as s2:
            CW = 3584
            cf = [s2.enter_context(nc.sbuf_tensor(U("cf"), [128, CW], F32)) for _ in range(2)]
            cb = s2.enter_context(nc.sbuf_tensor(U("cb"), [128, CW], BF16))
            cst = [P.stream(s2, "cst%d" % i) for i in range(2)]
            chunks = []
            for name, (rows, cols) in WSH.items():
                rs = rows // NCORES
                sh = nc.dram_tensor(name + "_sh", [rs, cols], BF16).ap()
                full = nc.dram_tensor(name + "_b", [rows, cols], BF16).ap()
                wb[name] = full
                lst = [(r0, min(128, rs - r0), c0, min(CW, cols - c0)) for r0 in range(0, rs, 128) for c0 in range(0, cols, CW)]
                for n_, (r0, rr, c0, cw) in enumerate(lst):
                    chunks.append((name, sh, full, r0, rr, c0, cw, n_ == len(lst) - 1))

            def issue(t):
                name, sh, full, r0, rr, c0, cw, _ = chunks[t]
                P.dma_async(cst[t % 2], cf[t % 2][:rr, :cw], wsh_in[name][r0:r0 + rr, c0:c0 + cw])

            issue(0)
            for t, (name, sh, full, r0, rr, c0, cw, is_last) in enumerate(chunks):
                if t + 1 < len(chunks):
                    issue(t + 1)
                P.wait_stream("act", cst[t % 2])
                P.op("act", lambda g: g.copy(out=cb[:rr, :cw], in_=cf[t % 2][:rr, :cw]))
                P.dma(sh[r0:r0 + rr, c0:c0 + cw], cb[:rr, :cw])
                if is_last:
                    P.allgather(full, sh)
            P.join_cc()

        ident = st.enter_context(nc.sbuf_tensor(U("ident"), [128, 128], BF16))
        ccs = st.enter_context(nc.sbuf_tensor(U("ccs"), [128, 4], F32))
        P.op("pool", lambda g: g.memset(ident[:], 0.0))
        P.op("pool", lambda g: g.affine_select(out=ident[:], in_=ident[:], pattern=[[-1, 128]],
                                               compare_op=ALU.not_equal, fill=1.0, base=0, channel_multiplier=1))
        P.dma(ccs[:], cc_in)

        def to_featmajor(src_d, dstT_d, rows, cols, src_dt=F32):
            nk = cols // 128
            with ExitStack() as s2:
                xf = s2.enter_context(nc.sbuf_tensor(U("tf_xf"), [128, 2048], src_dt))
                xb = s2.enter_context(nc.sbuf_tensor(U("tf_xb"), [128, 2048], BF16))
                oT = s2.enter_context(nc.sbuf_tensor(U("tf_oT"), [128, nk, 512], BF16))
                pt = s2.enter_context(nc.psum_tensor(U("tf_pt"), [128, 128], BF16))
                for r0 in range(0, rows, 512):
                    for j in range(4):
                        for c0 in range(0, cols, 2048):
                            cw = min(2048, cols - c0)
                            P.dma(xf[:, :cw], src_d[r0 + j * 128:r0 + (j + 1) * 128, c0:c0 + cw])
                            if src_dt == BF16:
                                src = xf
                            else:
                                P.op("dve", lambda g: g.tensor_copy(out=xb[:, :cw], in_=xf[:, :cw]))
                                src = xb
                            for kk in range(cw // 128):
                                P.op("pe", lambda g: g.transpose(pt[:], src[:, kk * 128:(kk + 1) * 128], ident[:]))
                                P.op("dve", lambda g: g.tensor_copy(out=oT[:, c0 // 128 + kk, j * 128:(j + 1) * 128],
                                                                   in_=pt[:]))
                    for k0 in range(0, nk, 8):
                        kn = min(8, nk - k0)
                        P.dma(dstT_d[k0 * 128:(k0 + kn) * 128, r0:r0 + 512].rearrange("(k p) n -> p k n", p=128),
                              oT[:, k0:k0 + kn, :])

        def linear_tok(xT_d, w_d, out_d, K, row_ranges, col_ranges, out_dt=F32):
            nk = K // 128
            with ExitStack() as s2:
                RG = 1024
                xT = s2.enter_context(nc.sbuf_tensor(U("lt_xT"), [128, nk, RG], BF16))
                wt = [s2.enter_context(nc.sbuf_tensor(U("lt_wt"), [128, nk, 512], BF16)) for _ in range(2)]
                ot = s2.enter_context(nc.sbuf_tensor(U("lt_ot"), [128, 512], out_dt))
                pm = s2.enter_context(nc.psum_tensor(U("lt_pm"), [128, 512], F32))
                wst = [P.stream(s2, U("lt_s")) for _ in range(2)]
                tiles = [(c0, min(512, cb_ - c0)) for (ca, cb_) in col_ranges for c0 in range(ca, cb_, 512)]

                def issue(t):
                    c0, cw = tiles[t]
                    for n_, k0 in enumerate(range(0, nk, 8)):
                        kn = min(8, nk - k0)
                        P.dma_async(wst[t % 2], wt[t % 2][:, k0:k0 + kn, :cw],
                                    w_d[k0 * 128:(k0 + kn) * 128, c0:c0 + cw].rearrange("(k p) n -> p k n", p=128), first=(n_ == 0))

                for (ra, rb) in row_ranges:
                    for g0 in range(ra, rb, RG):
                        gn = min(RG, rb - g0)
                        for k0 in range(0, nk, 8):
                            kn = min(8, nk - k0)
                            P.dma(xT[:, k0:k0 + kn, :gn],
                                  xT_d[k0 * 128:(k0 + kn) * 128, g0:g0 + gn].rearrange("(k p) n -> p k n", p=128))
                        issue(0)
                        for t, (c0, cw) in enumerate(tiles):
                            if t + 1 < len(tiles):
                                issue(t + 1)
                            P.wait_stream("pe", wst[t % 2])
                            w_ = wt[t % 2]
                            for j in range(gn // 128):
                                for k in range(nk):
                                    P.mm(pm[:, :cw], xT[:, k, j * 128:(j + 1) * 128], w_[:, k, :cw],
                                         start=(k == 0), stop=(k == nk - 1))
                                P.op("act", lambda g: g.copy(out=ot[:, :cw], in_=pm[:, :cw]))
                                P.dma(out_d[g0 + j * 128:g0 + (j + 1) * 128, c0:c0 + cw], ot[:, :cw])

        def ffn_up(xT_d, K, rows, w1_of, w3_of, F, hT_d, gateT_d=None):
            nk = K // 128
            with ExitStack() as s2:
                HT = 1024
                xT = s2.enter_context(nc.sbuf_tensor(U("fu_xT"), [128, nk, HT], BF16))
                w1h = [s2.enter_context(nc.sbuf_tensor(U("fu_w1"), [128, nk, 256], BF16)) for _ in range(2)]
                w3h = [s2.enter_context(nc.sbuf_tensor(U("fu_w3"), [128, nk, 256], BF16)) for _ in range(2)]
                gt = s2.enter_context(nc.sbuf_tensor(U("fu_gt"), [128, 512], BF16))
                ht = s2.enter_context(nc.sbuf_tensor(U("fu_ht"), [128, HT], BF16))
                gbc = s2.enter_context(nc.sbuf_tensor(U("fu_gbc"), [128, HT], F32))
                pa = s2.enter_context(nc.psum_tensor(U("fu_pa"), [128, 512], F32))
                pb = s2.enter_context(nc.psum_tensor(U("fu_pb"), [128, 512], F32))
                wst = [P.stream(s2, U("fu_s")) for _ in range(2)]
                items = [(fc, j) for fc in range(F // 512) for j in range(2)]

                def issue(t):
                    fc, j = items[t]
                    a1, a3 = w1_of(fc), w3_of(fc)
                    first = True
                    for k0 in range(0, nk, 8):
                        for (dst, a_) in ((w1h[t % 2], a1), (w3h[t % 2], a3)):
                            P.dma_async(wst[t % 2], dst[:, k0:k0 + 8, :],
                                        a_[k0 * 128:(k0 + 8) * 128, j * 256:(j + 1) * 256].rearrange("(k p) n -> p k n", p=128),
                                        first=first)
                            first = False

                for h0 in range(0, rows, HT):
                    for k0 in range(0, nk, 8):
                        P.dma(xT[:, k0:k0 + 8, :],
                              xT_d[k0 * 128:(k0 + 8) * 128, h0:h0 + HT].rearrange("(k p) n -> p k n", p=128))
                    cur_e = -1
                    issue(0)
                    for t, (fc, j) in enumerate(items):
                        if gateT_d is not None:
                            e = (fc * 512) // (F // 8)
                            if e != cur_e:
                                cur_e = e
                                P.dma(gbc[:], gateT_d[e:e + 1, h0:h0 + HT].partition_broadcast(128))
                        if t + 1 < len(items):
                            issue(t + 1)
                        P.wait_stream("pe", wst[t % 2])
                        w1t, w3t = w1h[t % 2], w3h[t % 2]
                        for s_ in range(2):
                            for tt in range(HT // 512):
                                tok = slice(tt * 512, (tt + 1) * 512)
                                for k in range(nk):
                                    P.mm(pa[:], w1t[:, k, s_ * 128:(s_ + 1) * 128], xT[:, k, tok],
                                         start=(k == 0), stop=(k == nk - 1))
                                for k in range(nk):
                                    P.mm(pb[:], w3t[:, k, s_ * 128:(s_ + 1) * 128], xT[:, k, tok],
                                         start=(k == 0), stop=(k == nk - 1))
                                P.op("act", lambda g: g.activation(out=gt[:], in_=pa[:], func=AF.Silu))
                                if gateT_d is None:
                                    P.op("dve", lambda g: g.tensor_tensor(out=ht[:, tok], in0=gt[:], in1=pb[:], op=ALU.mult))
                                else:
                                    P.op("dve", lambda g: g.tensor_tensor(out=gt[:], in0=gt[:], in1=pb[:], op=ALU.mult))
                                    P.op("dve", lambda g: g.tensor_tensor(out=ht[:, tok], in0=gt[:], in1=gbc[:, tok], op=ALU.mult))
                            m = fc * 4 + j * 2 + s_
                            P.dma(hT_d[m * 128:(m + 1) * 128, h0:h0 + HT], ht[:])

        def down_ln(hT_d, F, w2_d, res_d, g_in, b_in, out_d, y_d, w2_of=None):
            FC = F // 128
            with ExitStack() as s2:
                w2t = [s2.enter_context(nc.sbuf_tensor(U("dl_w2"), [128, 20, 512], BF16)) for _ in range(2)]
                hb = [s2.enter_context(nc.sbuf_tensor(U("dl_hb"), [128, 20, 512], BF16)) for _ in range(2)]
                if w2_of is None:
                    w2_of = lambda m0, mn: w2_d[m0 * 128:(m0 + mn) * 128, :]
                yo = s2.enter_context(nc.sbuf_tensor(U("dl_yo"), [128, 512], F32))
                py = [s2.enter_context(nc.psum_tensor(U("dl_py%d" % i), [128, 512], F32)) for i in range(4)]
                dst_ = [P.stream(s2, U("dl_s")) for _ in range(2)]
                items = [(ct, tq, m0) for ct in range(D // 512) for tq in range(NT // 512) for m0 in range(0, FC, 20)]

                def issue(t):
                    ct, tq, m0 = items[t]
                    mn = min(20, FC - m0)
                    P.dma_async(dst_[t % 2], w2t[t % 2][:, :mn, :],
                                w2_of(m0, mn)[:, ct * 512:(ct + 1) * 512].rearrange("(m p) n -> p m n", p=128))
                    P.dma_async(dst_[t % 2], hb[t % 2][:, :mn, :],
                                hT_d[m0 * 128:(m0 + mn) * 128, tq * 512:(tq + 1) * 512].rearrange("(m p) n -> p m n", p=128), first=False)

                issue(0)
                for t, (ct, tq, m0) in enumerate(items):
                    cols = slice(ct * 512, (ct + 1) * 512)
                    mn = min(20, FC - m0)
                    if t + 1 < len(items):
                        issue(t + 1)
                    P.wait_stream("pe", dst_[t % 2])
                    for j in range(4):
                        for mm_ in range(mn):
                            m = m0 + mm_
                            P.mm(py[j][:], hb[t % 2][:, mm_, j * 128:(j + 1) * 128], w2t[t % 2][:, mm_, :],
                                 start=(m == 0), stop=(m == FC - 1), chain=(mm_ > 0))
                    if m0 + mn == FC:
                        for j in range(4):
                            tb = tq * 4 + j
                            P.op("act", lambda g: g.copy(out=yo[:], in_=py[j][:]))
                            P.dma(y_d[tb * 128:(tb + 1) * 128, cols], yo[:])
            with ExitStack() as s2:
                sb = lambda name, shape, dt: s2.enter_context(nc.sbuf_tensor(U(name), shape, dt))
                gb_ = sb("dl_g", [128, D], F32); bb_ = sb("dl_b", [128, D], F32)
                xa = sb("dl_xa", [128, D], F32); ya = sb("dl_ya", [128, D], F32); sq = sb("dl_sq", [128, D], F32)
                stat = sb("dl_st", [128, 8], F32)
                P.dma(gb_[:], g_in.partition_broadcast(128))
                P.dma(bb_[:], b_in.partition_broadcast(128))
                for t in range(NB):
                    rows = slice(t * 128, (t + 1) * 128)
                    P.dma(xa[:], res_d[rows, :])
                    P.dma(ya[:], y_d[rows, :])
                    P.op("dve", lambda g: g.scalar_tensor_tensor(out=ya[:], in0=xa[:], scalar=ALPHA, in1=ya[:],
                                                                 op0=ALU.mult, op1=ALU.add))
                    layer_norm_rows(ya, sq, stat, D, LN_EPS)
                    P.op("dve", lambda g: g.tensor_tensor(out=ya[:], in0=ya[:], in1=gb_[:], op=ALU.mult))
                    P.op("dve", lambda g: g.tensor_tensor(out=ya[:], in0=ya[:], in1=bb_[:], op=ALU.add))
                    P.dma(out_d[rows, :], ya[:])

        def layer_norm_rows(ya, sq, stat, n, eps, center=True):
            v = ya[:, :n]
            if center:
                P.op("dve", lambda g: g.reduce_sum(out=stat[:, 0:1], in_=v, axis=AX.X))
                P.op("dve", lambda g: g.tensor_scalar(out=stat[:, 1:2], in0=stat[:, 0:1], scalar1=-1.0 / n,
                                                      scalar2=None, op0=ALU.mult))
                P.op("dve", lambda g: g.tensor_scalar(out=v, in0=v, scalar1=stat[:, 1:2], scalar2=None, op0=ALU.add))
            P.op("dve", lambda g: g.tensor_tensor(out=sq[:, :n], in0=v, in1=v, op=ALU.mult))
            P.op("dve", lambda g: g.reduce_sum(out=stat[:, 2:3], in_=sq[:, :n], axis=AX.X))
            P.op("dve", lambda g: g.tensor_scalar(out=stat[:, 3:4], in0=stat[:, 2:3], scalar1=1.0 / n,
                                                  scalar2=eps, op0=ALU.mult, op1=ALU.add))
            P.op("act", lambda g: g.sqrt(out=stat[:, 4:5], in_=stat[:, 3:4]))
            P.op("dve", lambda g: g.reciprocal(out=stat[:, 5:6], in_=stat[:, 4:5]))
            P.op("dve", lambda g: g.tensor_scalar(out=v, in0=v, scalar1=stat[:, 5:6], scalar2=None, op0=ALU.mult))

        def rope(x1, x2, cs, sn, t1, t2, t3):
            P.op("dve", lambda g: g.tensor_tensor(out=t1, in0=x1, in1=cs, op=ALU.mult))
            P.op("dve", lambda g: g.tensor_tensor(out=t2, in0=x2, in1=sn, op=ALU.mult))
            P.op("dve", lambda g: g.tensor_tensor(out=t3, in0=x1, in1=sn, op=ALU.mult))
            P.op("dve", lambda g: g.tensor_tensor(out=x1, in0=t1, in1=t2, op=ALU.subtract))
            P.op("dve", lambda g: g.tensor_tensor(out=t1, in0=x2, in1=cs, op=ALU.mult))
            P.op("dve", lambda g: g.tensor_tensor(out=x2, in0=t1, in1=t3, op=ALU.add))

        xT_d = scratch("xT_d", [D, T], BF16)
        to_featmajor(xp, xT_d, T, D)
        proj_d = scratch("proj_d", [T, C_END], F32)
        linear_tok(xT_d, wb["w_in"], proj_d, D, [(0, NT)], [(0, C_END)])
        linear_tok(xT_d, wb["w_in"], proj_d, D, [(NT, T)], [(C_KA, C_QI), (C_KI, C_WI), (C_KB, C_GB)])
        if upto == "proj":
            P.finish(); return nc

        qa_b = scratch("qa_b", [NT, 2048], BF16)
        ka_b = scratch("ka_b", [T, 512], BF16)
        va_d = scratch("va_d", [T, 512], BF16)
        qiP_b = scratch("qiP_b", [NT, 1024], BF16)
        qiN_b = scratch("qiN_b", [NT, 1024], BF16)
        ki_b = scratch("ki_b", [T, 128], BF16)
        qb_b = scratch("qb_b", [NT, 2048], BF16)
        kb_b = scratch("kb_b", [T, 2048], BF16)
        kbd_d = scratch("kbd_d", [T, 2048], BF16)
        vb_d = scratch("vb_d", [T, 2048], BF16)
        with ExitStack() as s2:
            sb = lambda name, shape, dt: s2.enter_context(nc.sbuf_tensor(U(name), shape, dt))
            xq = sb("pp_x", [128, 2048], F32)
            t1 = sb("pp_t1", [128, 1024], F32); t2 = sb("pp_t2", [128, 1024], F32); t3 = sb("pp_t3", [128, 1024], F32)
            ob = sb("pp_ob", [128, 2048], BF16); ob2 = sb("pp_ob2", [128, 2048], BF16)
            cA = sb("pp_cA", [128, 256], F32); sA = sb("pp_sA", [128, 256], F32)
            cI = sb("pp_cI", [128, 128], F32); sI = sb("pp_sI", [128, 128], F32)
            cR = sb("pp_cR", [128, 128], F32); sR = sb("pp_sR", [128, 128], F32)
            wi = sb("pp_wi", [128, 16], F32); wp = sb("pp_wp", [128, 16], F32); wn = sb("pp_wn", [128, 16], F32)
            rv = sb("pp_rv", [128, 32], F32)
            P.dma(rv[:], rvec_in)
            for blk in range(T // 128):
                rows = slice(blk * 128, (blk + 1) * 128)
                own = blk < NB
                P.dma(cA[:], cosA[rows, :]); P.dma(sA[:], sinA[rows, :])
                P.dma(cI[:], cosI[rows, :]); P.dma(sI[:], sinI[rows, :])
                P.dma(cR[:], cosR[rows, :]); P.dma(sR[:], sinR[rows, :])
                v3 = lambda ap, h: ap.rearrange("p (h d) -> p h d", h=h)
                if own:
                    P.dma(xq[:], proj_d[rows, C_QA:C_KA])
                    q3 = v3(xq[:], 16)
                    rope(q3[:, :, 0:16], q3[:, :, 16:32], v3(cA[:], 16), v3(sA[:], 16),
                         v3(t1[:, :256], 16), v3(t2[:, :256], 16), v3(t3[:, :256], 16))
                    P.op("act", lambda g: g.activation(out=ob[:], in_=xq[:], func=AF.Copy, scale=128.0 ** -0.5))
                    P.dma(qa_b[rows, :], ob[:])
                P.dma(xq[:, :512], proj_d[rows, C_KA:C_VA])
                k3 = v3(xq[:, :512], 4)
                rope(k3[:, :, 0:16], k3[:, :, 16:32], v3(cA[:, :64], 4), v3(sA[:, :64], 4),
                     v3(t1[:, :64], 4), v3(t2[:, :64], 4), v3(t3[:, :64], 4))
                P.op("act", lambda g: g.copy(out=ob[:, :512], in_=xq[:, :512]))
                P.dma(ka_b[rows, :], ob[:, :512])
                P.dma(xq[:, :512], proj_d[rows, C_VA:C_QI])
                P.op("act", lambda g: g.copy(out=ob[:, :512], in_=xq[:, :512]))
                P.dma(va_d[rows, :], ob[:, :512])
                if own:
                    P.dma(xq[:, :1024], proj_d[rows, C_QI:C_KI])
                    P.dma(wi[:], proj_d[rows, C_WI:C_QB])
                    q3 = v3(xq[:, :1024], 16)
                    rope(q3[:, :, 0:8], q3[:, :, 8:16], v3(cI[:], 16), v3(sI[:], 16),
                         v3(t1[:, :128], 16), v3(t2[:, :128], 16), v3(t3[:, :128], 16))
                    P.op("dve", lambda g: g.tensor_scalar(out=wp[:], in0=wi[:], scalar1=0.0, scalar2=None, op0=ALU.max))
                    P.op("dve", lambda g: g.tensor_scalar(out=wn[:], in0=wi[:], scalar1=0.0, scalar2=None, op0=ALU.min))
                    for hh in range(16):
                        P.op("dve", lambda g: g.tensor_scalar(out=ob[:, hh * 64:(hh + 1) * 64], in0=xq[:, hh * 64:(hh + 1) * 64],
                                                              scalar1=wp[:, hh:hh + 1], scalar2=None, op0=ALU.mult))
                        P.op("dve", lambda g: g.tensor_scalar(out=ob2[:, hh * 64:(hh + 1) * 64], in0=xq[:, hh * 64:(hh + 1) * 64],
                                                              scalar1=wn[:, hh:hh + 1], scalar2=None, op0=ALU.mult))
                    P.dma(qiP_b[rows, :], ob[:, :1024])
                    P.dma(qiN_b[rows, :], ob2[:, :1024])
                P.dma(xq[:, :64], proj_d[rows, C_KI:C_WI])
                rope(xq[:, 0:8], xq[:, 8:16], cI[:, 0:8], sI[:, 0:8], t1[:, :8], t2[:, :8], t3[:, :8])
                P.op("act", lambda g: g.copy(out=ob[:, 0:64], in_=xq[:, :64]))
                P.op("act", lambda g: g.copy(out=ob[:, 64:128], in_=xq[:, :64]))
                P.dma(ki_b[rows, :], ob[:, :128])
                for (c0, dst, is_q) in (((C_QB, qb_b, True),) if own else ()) + ((C_KB, kb_b, False),):
                    P.dma(xq[:], proj_d[rows, c0:c0 + 2048])
                    for hh in range(8):
                        rope(xq[:, hh * 256:hh * 256 + 128], xq[:, hh * 256 + 128:(hh + 1) * 256], cR[:], sR[:],
                             t1[:, :128], t2[:, :128], t3[:, :128])
                    if is_q:
                        P.op("act", lambda g: g.copy(out=ob[:], in_=xq[:]))
                        P.dma(dst[rows, :], ob[:])
                    else:
                        P.op("act", lambda g: g.activation(out=ob[:], in_=xq[:], func=AF.Copy, scale=256.0 ** -0.5))
                        P.dma(dst[rows, :], ob[:])
                        for hh in range(8):
                            P.op("dve", lambda g: g.tensor_scalar(out=ob2[:, hh * 256:(hh + 1) * 256], in0=ob[:, hh * 256:(hh + 1) * 256],
                                                                  scalar1=rv[:, hh:hh + 1], scalar2=None, op0=ALU.mult))
                        P.dma(kbd_d[rows, :], ob2[:])
                P.dma(xq[:], proj_d[rows, C_VB:C_GB])
                P.op("act", lambda g: g.copy(out=ob[:], in_=xq[:]))
                P.dma(vb_d[rows, :], ob[:])
        qaT_d = scratch("qaT_d", [2048, NT], BF16); to_featmajor(qa_b, qaT_d, NT, 2048, BF16)
        kaT_d = scratch("kaT_d", [512, T], BF16); to_featmajor(ka_b, kaT_d, T, 512, BF16)
        qiPT_d = scratch("qiPT_d", [1024, NT], BF16); to_featmajor(qiP_b, qiPT_d, NT, 1024, BF16)
        qiNT_d = scratch("qiNT_d", [1024, NT], BF16); to_featmajor(qiN_b, qiNT_d, NT, 1024, BF16)
        kiT_d = scratch("kiT_d", [128, T], BF16); to_featmajor(ki_b, kiT_d, T, 128, BF16)
        qbT_d = scratch("qbT_d", [2048, NT], BF16); to_featmajor(qb_b, qbT_d, NT, 2048, BF16)
        kbT_d = scratch("kbT_d", [2048, T], BF16); to_featmajor(kb_b, kbT_d, T, 2048, BF16)
        if upto == "post":
            P.finish(); return nc

        maskT_d = scratch("maskT_d", [T, NT], BF16)
        with ExitStack() as s2:
            sb = lambda name, shape, dt: s2.enter_context(nc.sbuf_tensor(U(name), shape, dt))
            ps = lambda name, shape, dt: s2.enter_context(nc.psum_tensor(U(name), shape, dt))
            kiT = sb("ix_kiT", [128, T], BF16)
            qP = sb("ix_qP", [128, 8, 128], BF16); qN = sb("ix_qN", [128, 8, 128], BF16)
            acc = sb("ix_acc", [128, T], F32)
            work = sb("ix_work", [128, T], F32)
            pinit = sb("ix_pinit", [128, NT], F32)
            negtri = sb("ix_negtri", [128, 128], F32)
            mx = sb("ix_mx", [128, 8], F32)
            thr = sb("ix_thr", [128, 1], F32)
            mk = sb("ix_mk", [128, T], BF16)
            mT = sb("ix_mT", [128, 32, 128], BF16)
            pl = ps("ix_pl", [128, 512], F32)
            pt = ps("ix_pt", [128, 128], BF16)
            P.dma(kiT[:], kiT_d[:, :])
            P.dma(negtri[:], negtri_in)
            P.op("pool", lambda g: g.memset(pinit[:], 0.0))
            P.op("dve", lambda g: g.tensor_scalar(out=pinit[:], in0=pinit[:], scalar1=ccs[:, 0:1], scalar2=None, op0=ALU.add))
            for i in range(NB):
                q0 = i * 128
                P.dma(qP[:], qiPT_d[:, q0:q0 + 128].rearrange("(k p) n -> p k n", p=128))
                P.dma(qN[:], qiNT_d[:, q0:q0 + 128].rearrange("(k p) n -> p k n", p=128))
                nown = (i + 1) * 128
                P.op("pool", lambda g: g.memset(acc[:, 0:NT], 0.0))
                if nown < NT:
                    P.op("pool", lambda g: g.memset(acc[:, nown:NT], NEG))
                P.op("dve", lambda g: g.tensor_copy(out=acc[:, q0:q0 + 128], in_=negtri[:]))
                P.op("dve", lambda g: g.tensor_copy(out=acc[:, NT:T], in_=pinit[:]))
                tiles = [(k0, min(512, nown - k0)) for k0 in range(0, nown, 512)] + [(NT + k0, 512) for k0 in range(0, NT, 512)]
                for (k0, kw) in tiles:
                    for hh in range(16):
                        pr = slice((hh % 2) * 64, (hh % 2) * 64 + 64)
                        for (qt_, op0) in ((qP, ALU.max), (qN, ALU.min)):
                            P.mm(pl[:, :kw], qt_[pr, hh // 2, :], kiT[pr, k0:k0 + kw], start=True, stop=True)
                            P.op("dve", lambda g: g.scalar_tensor_tensor(out=acc[:, k0:k0 + kw], in0=pl[:, :kw], scalar=0.0,
                                                                         in1=acc[:, k0:k0 + kw], op0=op0, op1=ALU.add))
                P.op("dve", lambda g: g.tensor_copy(out=work[:], in_=acc[:]))
                for r in range(32):
                    P.op("dve", lambda g: g.max(out=mx[:], in_=work[:]))
                    if r < 31:
                        P.op("dve", lambda g: g.match_replace(out=work[:], in_to_replace=mx[:], in_values=work[:], imm_value=-3.0e38))
                P.op("dve", lambda g: g.tensor_scalar(out=thr[:], in0=mx[:, 7:8], scalar1=-1.0e29, scalar2=None, op0=ALU.max))
                P.op("dve", lambda g: g.tensor_scalar(out=mk[:], in0=acc[:], scalar1=thr[:, 0:1], scalar2=None, op0=ALU.is_ge))
                kbl = list(range(i + 1)) + list(range(NB, 2 * NB))
                for kb in kbl:
                    P.op("pe", lambda g: g.transpose(pt[:], mk[:, kb * 128:(kb + 1) * 128], ident[:]))
                    P.op("act", lambda g: g.copy(out=mT[:, kb, :], in_=pt[:]))
                if i + 1 < NB:
                    P.op("pool", lambda g: g.memset(mT[:, i + 1:NB, :], 0.0))
                P.dma(maskT_d[:, q0:q0 + 128].rearrange("(k p) n -> p k n", p=128), mT[:])
        if upto == "index":
            P.finish(); return nc

        ymixT_d = scratch("ymixT_d", [D, NT], BF16)

        def attention(nheads, kv_of, load_k, load_q, load_v, mask_mode, outT_d, out_row0):
            with ExitStack() as s2:
                sb = lambda name, shape, dt: s2.enter_context(nc.sbuf_tensor(U(name), shape, dt))
                ps = lambda name, shape, dt: s2.enter_context(nc.psum_tensor(U(name), shape, dt))
                k1 = sb("at_k1", [128, T], BF16); k2 = sb("at_k2", [64, T], BF16)
                q1 = sb("at_q1", [128, 512], BF16); q2 = sb("at_q2", [64, 512], BF16)
                vt = sb("at_v", [128, 32, 128], BF16)
                mT = sb("at_mT", [128, 32, 512], BF16)
                pT = sb("at_pT", [128, 512], BF16)
                ones_c = sb("at_1c", [128, 1], BF16); ones_r = sb("at_1r", [1, 128], F32)
                rl = sb("at_rl", [1, 512], F32); bc = sb("at_bc", [128, 512], F32)
                oT = sb("at_oT", [128, 512], BF16)
                triT = sb("at_tri", [128, 128], F32); triTb = sb("at_trib", [128, 128], BF16)
                cm = sb("at_cm", [128, 4, 512], BF16)
                pS = ps("at_pS", [128, 512], F32); pO = ps("at_pO", [128, 512], F32)
                pL = ps("at_pL", [1, 512], F32); pB = ps("at_pB", [128, 512], F32)
                P.op("pool", lambda g: g.memset(ones_c[:], 1.0))
                P.op("pool", lambda g: g.memset(ones_r[:], 1.0))
                if mask_mode == "causal":
                    P.dma(triT[:], triT_in)
                    P.op("dve", lambda g: g.tensor_copy(out=triTb[:], in_=triT[:]))
                    for j in range(4):
                        if j > 0:
                            P.op("pool", lambda g: g.memset(cm[:, j, 0:j * 128], 0.0))
                        P.op("dve", lambda g: g.tensor_copy(out=cm[:, j, j * 128:(j + 1) * 128], in_=triTb[:]))
                        if j < 3:
                            P.op("pool", lambda g: g.memset(cm[:, j, (j + 1) * 128:512], 1.0))
                cur_g = -1
                for qt in range(NT // 512):
                    qs = slice(qt * 512, (qt + 1) * 512)
                    if mask_mode == "dsa":
                        for k0 in range(0, 32, 8):
                            P.dma(mT[:, k0:k0 + 8, :], maskT_d[k0 * 128:(k0 + 8) * 128, qs].rearrange("(k p) n -> p k n", p=128))
                    for head in range(nheads):
                        g_ = kv_of(head)
                        if g_ != cur_g:
                            cur_g = g_
                            kparts = load_k(g_, k1, k2)
                            load_v(g_, vt)
                        qparts = load_q(head, qs, q1, q2)
                        kbl = list(range(4 * qt + 4)) + list(range(NB, 2 * NB))
                        for n_, kb in enumerate(kbl):
                            ks = slice(kb * 128, (kb + 1) * 128)
                            for pi, (kt_, qt_) in enumerate(zip(kparts, qparts)):
                                P.mm(pS[:], kt_[:, ks], qt_[:, :], start=(pi == 0), stop=(pi == len(kparts) - 1))
                            if kb >= NB and mask_mode == "causal":
                                P.op("act", lambda g: g.activation(out=pT[:], in_=pS[:], func=AF.Exp, bias=ccs[:, 1:2]))
                            else:
                                P.op("act", lambda g: g.activation(out=pT[:], in_=pS[:], func=AF.Exp))
                            if mask_mode == "dsa":
                                P.op("dve", lambda g: g.tensor_tensor(out=pT[:], in0=pT[:], in1=mT[:, kb, :], op=ALU.mult))
                            elif kb < NB and kb >= 4 * qt:
                                P.op("dve", lambda g: g.tensor_tensor(out=pT[:], in0=pT[:], in1=cm[:, kb - 4 * qt, :], op=ALU.mult))
                            first, last_ = (n_ == 0), (n_ == len(kbl) - 1)
                            P.op("pe", lambda g: g.matmul(pO[:], vt[:, kb, :], pT[:], start=first, stop=last_))
                            P.op("pe", lambda g: g.matmul(pL[:], ones_c[:], pT[:], start=first, stop=last_), chain=True)
                        P.op("dve", lambda g: g.reciprocal(out=rl[:], in_=pL[:]))
                        P.mm(pB[:], ones_r[:], rl[:], start=True, stop=True)
                        P.op("act", lambda g: g.copy(out=bc[:], in_=pB[:]))
                        P.op("dve", lambda g: g.tensor_tensor(out=oT[:], in0=pO[:], in1=bc[:], op=ALU.mult))
                        P.dma(outT_d[out_row0 + head * 128:out_row0 + (head + 1) * 128, qs], oT[:])

        def dsa_load_k(g_, k1, k2):
            P.dma(k1[:], kaT_d[g_ * 128:(g_ + 1) * 128, :])
            return [k1]

        def dsa_load_q(head, qs, q1, q2):
            P.dma(q1[:], qaT_d[head * 128:(head + 1) * 128, qs])
            return [q1]

        def dsa_load_v(g_, vt):
            P.dma(vt[:], va_d[:, g_ * 128:(g_ + 1) * 128].rearrange("(k p) n -> p k n", p=128))

        attention(16, lambda hd: hd // 4, dsa_load_k, dsa_load_q, dsa_load_v, "dsa", ymixT_d, 0)
        if upto == "dsa":
            P.finish(); return nc

        with ExitStack() as s2:
            sb = lambda name, shape, dt: s2.enter_context(nc.sbuf_tensor(U(name), shape, dt))
            ps = lambda name, shape, dt: s2.enter_context(nc.psum_tensor(U(name), shape, dt))
            S = sb("rt_S", [128, 2, 256], F32); Sb = sb("rt_Sb", [128, 2, 256], BF16)
            kd = sb("rt_kd", [128, 256], BF16); vv = sb("rt_v", [128, 256], BF16)
            qT = sb("rt_qT", [128, 2, 128], BF16); kT = sb("rt_kT", [128, 2, 128], BF16)
            rdec = sb("rt_rdec", [128, 1024], F32); rv = sb("rt_rv", [128, 32], F32)
            PT = sb("rt_PT", [128, 128], BF16)
            O = sb("rt_O", [128, 256], F32); sq = sb("rt_sq", [128, 256], F32); stat = sb("rt_st", [128, 8], F32)
            gng = sb("rt_gng", [128, 2048], F32); gt = sb("rt_g", [128, 256], F32)
            ob = sb("rt_ob", [128, 256], BF16); yT = sb("rt_yT", [128, 2, 128], BF16)
            pU = [ps("rt_pU%d" % i, [128, 512], F32)[:, 0:256] for i in range(2)]
            pST = ps("rt_pST", [128, 512], F32)[:, 0:128]; pOi = ps("rt_pOi", [128, 512], F32)[:, 0:256]
            pQS = ps("rt_pQS", [128, 512], F32)[:, 0:256]
            pt = ps("rt_pt", [128, 1024], BF16)[:, 0:128]
            P.dma(rdec[:], rdecT_in); P.dma(rv[:], rvec_in)
            P.dma(gng[:], gn_g.partition_broadcast(128))

            def state_update(rows, hh):
                hc = slice(hh * 256, (hh + 1) * 256)
                P.dma(kd[:], kbd_d[rows, hc]); P.dma(vv[:], vb_d[rows, hc])
                for dc in range(2):
                    P.mm(pU[dc][:], kd[:, dc * 128:(dc + 1) * 128], vv[:], start=True, stop=True)
                    P.op("dve", lambda g: g.scalar_tensor_tensor(out=S[:, dc, :], in0=S[:, dc, :], scalar=rv[:, 16 + hh:17 + hh],
                                                                 in1=pU[dc][:], op0=ALU.mult, op1=ALU.add))

            for hh in range(8):
                hc = slice(hh * 256, (hh + 1) * 256)
                P.op("pool", lambda g: g.memset(S[:], 0.0))
                for i in range(NB):
                    state_update(slice(NT + i * 128, NT + (i + 1) * 128), hh)
                P.op("dve", lambda g: g.tensor_scalar(out=S[:], in0=S[:], scalar1=ccs[:, 2:3], scalar2=None, op0=ALU.mult))
                for i in range(NB):
                    rows = slice(i * 128, (i + 1) * 128)
                    P.op("act", lambda g: g.copy(out=Sb[:], in_=S[:]))
                    P.dma(qT[:], qbT_d[hh * 256:(hh + 1) * 256, rows].rearrange("(k p) n -> p k n", p=128))
                    P.dma(kT[:], kbT_d[hh * 256:(hh + 1) * 256, rows].rearrange("(k p) n -> p k n", p=128))
                    P.dma(vv[:], vb_d[rows, hc])
                    for dc in range(2):
                        P.mm(pST[:], kT[:, dc, :], qT[:, dc, :], start=(dc == 0), stop=(dc == 1))
                    P.op("dve", lambda g: g.tensor_tensor(out=PT[:], in0=pST[:], in1=rdec[:, hh * 128:(hh + 1) * 128], op=ALU.mult))
                    P.mm(pOi[:], PT[:], vv[:], start=True, stop=True)
                    for dc in range(2):
                        P.mm(pQS[:], qT[:, dc, :], Sb[:, dc, :], start=(dc == 0), stop=(dc == 1))
                    P.op("act", lambda g: g.copy(out=O[:], in_=pOi[:]))
                    P.op("dve", lambda g: g.scalar_tensor_tensor(out=O[:], in0=pQS[:], scalar=rv[:, 8 + hh:9 + hh], in1=O[:],
                                                                 op0=ALU.mult, op1=ALU.add))
                    layer_norm_rows(O, sq, stat, 256, LN_EPS)
                    P.dma(gt[:], proj_d[rows, C_GB + hh * 256:C_GB + (hh + 1) * 256])
                    P.op("act", lambda g: g.activation(out=gt[:], in_=gt[:], func=AF.Silu))
                    P.op("dve", lambda g: g.tensor_tensor(out=O[:], in0=O[:], in1=gng[:, hc], op=ALU.mult))
                    P.op("dve", lambda g: g.tensor_tensor(out=ob[:], in0=O[:], in1=gt[:], op=ALU.mult))
                    for dc in range(2):
                        P.op("pe", lambda g: g.transpose(pt[:], ob[:, dc * 128:(dc + 1) * 128], ident[:]))
                        P.op("act", lambda g: g.copy(out=yT[:, dc, :], in_=pt[:]))
                    P.dma(ymixT_d[2048 + hh * 256:2048 + (hh + 1) * 256, rows].rearrange("(k p) n -> p k n", p=128), yT[:])
                    state_update(rows, hh)
        if upto == "ret":
            P.finish(); return nc

        y_d = scratch("y_d", [NT, D], F32)
        x1_d = scratch("x1_d", [NT, D], F32)
        down_ln(ymixT_d, D, wb["w_out0"], xp, ln_p["l0_ln1_g"], ln_p["l0_ln1_b"], x1_d, y_d)
        x1T_d = scratch("x1T_d", [D, NT], BF16)
        to_featmajor(x1_d, x1T_d, NT, D)
        hT_d = scratch("hT_d", [40960, NT], BF16)
        ffn_up(x1T_d, D, NT, lambda fc: wb["f_w1"][:, fc * 512:(fc + 1) * 512], lambda fc: wb["f_w3"][:, fc * 512:(fc + 1) * 512],
               FFN, hT_d)
        x2_d = out if upto == "l0" else scratch("x2_d", [NT, D], F32)
        down_ln(hT_d, FFN, wb["f_w2"], x1_d, ln_p["l0_ln2_g"], ln_p["l0_ln2_b"], x2_d, y_d)
        if upto == "l0":
            P.finish(); return nc

        x2T_d = scratch("x2T_d", [D, NT], BF16)
        to_featmajor(x2_d, x2T_d, NT, D)
        c_d = scratch("c_d", [NT, 1600], F32)
        linear_tok(x2T_d, wb["w_dq"], c_d, D, [(0, NT)], [(0, 1600)])
        cqn_b = scratch("cqn_b", [NT, 1024], BF16)
        LAT_d = scratch("LAT_d", [T, 640], BF16)
        SH = nc.dram_tensor("lat_shared", [T, 640], BF16, addr_space="Shared").ap()
        with ExitStack() as s2:
            sb = lambda name, shape, dt: s2.enter_context(nc.sbuf_tensor(U(name), shape, dt))
            ct = sb("l1_c", [128, 1600], F32); sq = sb("l1_sq", [128, 1024], F32); stat = sb("l1_st", [128, 8], F32)
            qg = sb("l1_qg", [128, 1024], F32); kg = sb("l1_kg", [128, 512], F32)
            cq_b = sb("l1_cqb", [128, 1024], BF16); lat = sb("l1_lat", [128, 640], BF16)
            cC = sb("l1_cC", [128, 32], F32); sC = sb("l1_sC", [128, 32], F32)
            t1 = sb("l1_t1", [128, 32], F32); t2 = sb("l1_t2", [128, 32], F32); t3 = sb("l1_t3", [128, 32], F32)
            widx = sb("l1_widx", [128, 16], mybir.dt.int32); ridx = sb("l1_ridx", [128, 16], mybir.dt.int32)
            P.dma(qg[:], qn_g.partition_broadcast(128)); P.dma(kg[:], kvn_g.partition_broadcast(128))
            P.dma(widx[:], widx_in); P.dma(ridx[:], ridx_in)
            P.op("pool", lambda g: g.memset(lat[:], 0.0))
            for i in range(NB):
                rows = slice(i * 128, (i + 1) * 128)
                P.dma(ct[:], c_d[rows, :])
                P.dma(cC[:], cosC[rows, :]); P.dma(sC[:], sinC[rows, :])
                layer_norm_rows(ct[:, 0:1024], sq, stat, 1024, RMS_EPS, center=False)
                P.op("dve", lambda g: g.tensor_tensor(out=cq_b[:], in0=ct[:, 0:1024], in1=qg[:], op=ALU.mult))
                P.dma(cqn_b[rows, :], cq_b[:])
                layer_norm_rows(ct[:, 1024:1536], sq, stat, 512, RMS_EPS, center=False)
                P.op("dve", lambda g: g.tensor_tensor(out=lat[:, 0:512], in0=ct[:, 1024:1536], in1=kg[:], op=ALU.mult))
                rope(ct[:, 1536:1568], ct[:, 1568:1600], cC[:], sC[:], t1[:], t2[:], t3[:])
                P.op("act", lambda g: g.copy(out=lat[:, 512:576], in_=ct[:, 1536:1600]))
                P.dma(LAT_d[rows, :], lat[:])
                P.op("pool", lambda g: g.indirect_dma_start(
                    out=SH[:, :], out_offset=bass.IndirectOffsetOnAxis(ap=widx[:, i:i + 1], axis=0),
                    in_=lat[:, :], in_offset=None, bounds_check=T - 1, oob_is_err=False), inc=16)
            P.finish()
            nc.all_core_barrier()
            for i in range(NB):
                P.op("pool", lambda g: g.indirect_dma_start(
                    out=lat[:, :], out_offset=None, in_=SH[:, :],
                    in_offset=bass.IndirectOffsetOnAxis(ap=ridx[:, i:i + 1], axis=0),
                    bounds_check=T - 1, oob_is_err=False), inc=16)
                P.dma(LAT_d[NT + i * 128:NT + (i + 1) * 128, :], lat[:])
        cqnT_d = scratch("cqnT_d", [1024, NT], BF16); to_featmajor(cqn_b, cqnT_d, NT, 1024, BF16)
        latT_d = scratch("latT_d", [640, T], BF16); to_featmajor(LAT_d, latT_d, T, 640, BF16)
        qf_d = scratch("qf_d", [NT, 6144], F32)
        linear_tok(cqnT_d, wb["w_uq"], qf_d, 1024, [(0, NT)], [(0, 6144)])
        kv_d = scratch("kv_d", [T, 8192], BF16)
        linear_tok(latT_d, wb["w_ukv"], kv_d, 512, [(0, T)], [(0, 8192)], out_dt=BF16)
        kvT_d = scratch("kvT_d", [8192, T], BF16); to_featmajor(kv_d, kvT_d, T, 8192, BF16)
        qn_b = scratch("qn_b", [NT, 4096], BF16); qr_b = scratch("qr_b", [NT, 2048], BF16)
        with ExitStack() as s2:
            sb = lambda name, shape, dt: s2.enter_context(nc.sbuf_tensor(U(name), shape, dt))
            qf = sb("l1_qf", [128, 6144], F32)
            qnb = sb("l1_qnb", [128, 4096], BF16); qrb = sb("l1_qrb", [128, 2048], BF16)
            cC = sb("l1_cC2", [128, 32], F32); sC = sb("l1_sC2", [128, 32], F32)
            t1 = sb("l1_u1", [128, 32], F32); t2 = sb("l1_u2", [128, 32], F32); t3 = sb("l1_u3", [128, 32], F32)
            for i in range(NB):
                rows = slice(i * 128, (i + 1) * 128)
                P.dma(qf[:], qf_d[rows, :])
                P.dma(cC[:], cosC[rows, :]); P.dma(sC[:], sinC[rows, :])
                for hd in range(32):
                    b0 = hd * 192 + 128
                    rope(qf[:, b0:b0 + 32], qf[:, b0 + 32:b0 + 64], cC[:], sC[:], t1[:], t2[:], t3[:])
                q3 = qf[:].rearrange("p (h d) -> p h d", h=32)
                P.op("act", lambda g: g.activation(out=qnb[:].rearrange("p (h d) -> p h d", h=32), in_=q3[:, :, 0:128],
                                                   func=AF.Copy, scale=192.0 ** -0.5))
                P.op("act", lambda g: g.activation(out=qrb[:].rearrange("p (h d) -> p h d", h=32), in_=q3[:, :, 128:192],
                                                   func=AF.Copy, scale=192.0 ** -0.5))
                P.dma(qn_b[rows, :], qnb[:]); P.dma(qr_b[rows, :], qrb[:])
        qnT_d = scratch("qnT_d", [4096, NT], BF16); to_featmajor(qn_b, qnT_d, NT, 4096, BF16)
        qrT_d = scratch("qrT_d", [2048, NT], BF16); to_featmajor(qr_b, qrT_d, NT, 2048, BF16)

        def mla_load_k(hd, k1, k2):
            P.dma(k1[:], kvT_d[hd * 256:hd * 256 + 128, :])
            P.dma(k2[:], latT_d[512:576, :])
            return [k1, k2]

        def mla_load_q(hd, qs, q1, q2):
            P.dma(q1[:], qnT_d[hd * 128:(hd + 1) * 128, qs])
            P.dma(q2[:], qrT_d[hd * 64:(hd + 1) * 64, qs])
            return [q1, q2]

        def mla_load_v(hd, vt):
            P.dma(vt[:], kv_d[:, hd * 256 + 128:(hd + 1) * 256].rearrange("(k p) n -> p k n", p=128))

        mlaT_d = scratch("mlaT_d", [D, NT], BF16)
        attention(32, lambda hd: hd, mla_load_k, mla_load_q, mla_load_v, "causal", mlaT_d, 0)
        x3_d = scratch("x3_d", [NT, D], F32)
        down_ln(mlaT_d, D, wb["w_out1"], x2_d, ln_p["l1_ln1_g"], ln_p["l1_ln1_b"], x3_d, y_d)
        if upto == "mla":
            P.finish(); return nc

        gateT_d = scratch("gateT_d", [8, NT], F32)
        with ExitStack() as s2:
            sb = lambda name, shape, dt: s2.enter_context(nc.sbuf_tensor(U(name), shape, dt))
            rb = sb("rt_rb", [128, D], F32); xb_ = sb("rt_xb", [128, D], F32); pr = sb("rt_pr", [128, D], F32)
            lg = sb("rt_lg", [128, NB, 8], F32)
            eq1 = sb("rt_eq1", [128, 8], F32); lg2 = sb("rt_lg2", [128, 8], F32); sel = sb("rt_sel", [128, 8], F32)
            gts = sb("rt_gts", [128, 8], F32); st8 = sb("rt_st8", [128, 8], F32)
            identF = sb("rt_idF", [128, 128], F32); gT = sb("rt_gT", [8, 128], F32)
            pg = s2.enter_context(nc.psum_tensor(U("rt_pg"), [128, 512], F32))
            P.op("pool", lambda g: g.memset(identF[:], 0.0))
            P.op("pool", lambda g: g.affine_select(out=identF[:], in_=identF[:], pattern=[[-1, 128]],
                                                   compare_op=ALU.not_equal, fill=1.0, base=0, channel_multiplier=1))
            for e in range(8):
                P.dma(rb[:], routerT[e:e + 1, :].partition_broadcast(128))
                for i in range(NB):
                    P.dma(xb_[:], x3_d[i * 128:(i + 1) * 128, :])
                    P.op("dve", lambda g: g.tensor_tensor(out=pr[:], in0=xb_[:], in1=rb[:], op=ALU.mult))
                    P.op("dve", lambda g: g.reduce_sum(out=lg[:, i, e:e + 1], in_=pr[:], axis=AX.X))
            for i in range(NB):
                l = lg[:, i, :]
                P.op("dve", lambda g: g.reduce_max(out=st8[:, 0:1], in_=l, axis=AX.X))
                P.op("dve", lambda g: g.tensor_scalar(out=eq1[:], in0=l, scalar1=st8[:, 0:1], scalar2=None, op0=ALU.is_equal))
                P.op("dve", lambda g: g.scalar_tensor_tensor(out=lg2[:], in0=eq1[:], scalar=NEG, in1=l, op0=ALU.mult, op1=ALU.add))
                P.op("dve", lambda g: g.reduce_max(out=st8[:, 1:2], in_=lg2[:], axis=AX.X))
                P.op("dve", lambda g: g.tensor_scalar(out=sel[:], in0=l, scalar1=st8[:, 1:2], scalar2=None, op0=ALU.is_ge))
                P.op("dve", lambda g: g.tensor_tensor(out=st8[:, 2:3], in0=st8[:, 1:2], in1=st8[:, 0:1], op=ALU.subtract))
                P.op("act", lambda g: g.activation(out=st8[:, 3:4], in_=st8[:, 2:3], func=AF.Exp))
                P.op("dve", lambda g: g.tensor_scalar(out=st8[:, 4:5], in0=st8[:, 3:4], scalar1=1.0, scalar2=None, op0=ALU.add))
                P.op("dve", lambda g: g.reciprocal(out=st8[:, 5:6], in_=st8[:, 4:5]))
                P.op("dve", lambda g: g.tensor_tensor(out=st8[:, 6:7], in0=st8[:, 3:4], in1=st8[:, 5:6], op=ALU.mult))
                P.op("dve", lambda g: g.tensor_tensor(out=st8[:, 7:8], in0=st8[:, 5:6], in1=st8[:, 6:7], op=ALU.subtract))
                P.op("dve", lambda g: g.tensor_scalar(out=gts[:], in0=eq1[:], scalar1=st8[:, 7:8], scalar2=None, op0=ALU.mult))
                P.op("dve", lambda g: g.scalar_tensor_tensor(out=gts[:], in0=sel[:], scalar=st8[:, 6:7], in1=gts[:],
                                                             op0=ALU.mult, op1=ALU.add))
                P.op("pe", lambda g: g.transpose(pg[0:8, 0:128], gts[:], identF[:]))
                P.op("act", lambda g: g.copy(out=gT[:], in_=pg[0:8, 0:128]))
                P.dma(gateT_d[:, i * 128:(i + 1) * 128], gT[:])

        x3T_d = scratch("x3T_d", [D, NT], BF16)
        to_featmajor(x3_d, x3T_d, NT, D)
        ffn_up(x3T_d, D, NT,
               lambda fc: wb["m_w1a" if fc % 10 < 5 else "m_w1b"][(fc // 10) * D:(fc // 10 + 1) * D, (fc % 5) * 512:(fc % 5 + 1) * 512],
               lambda fc: wb["m_w3a" if fc % 10 < 5 else "m_w3b"][(fc // 10) * D:(fc // 10 + 1) * D, (fc % 5) * 512:(fc % 5 + 1) * 512],
               40960, hT_d, gateT_d)

        def moe_w2_of(m0, mn):
            e, j = m0 // 40, m0 % 40
            piece = wb["m_w2a" if j < 20 else "m_w2b"]
            r0 = e * 2560 + (j % 20) * 128
            return piece[r0:r0 + mn * 128, :]

        down_ln(hT_d, 40960, None, x3_d, ln_p["l1_ln2_g"], ln_p["l1_ln2_b"], out, y_d, w2_of=moe_w2_of)

        P.finish()
    return nc


def _rope_tab(pos, rot_dim, theta):
    inv = theta ** (-np.arange(0, rot_dim, 2, dtype=np.float32) / rot_dim)
    ang = pos.astype(np.float32)[:, None] * inv[None, :].astype(np.float32)
    return np.cos(ang).astype(np.float32), np.sin(ang).astype(np.float32)


def _core_constants(c):
    h = c % 2
    pos = np.concatenate([np.arange(NT) + h * NT, np.arange(NT) + (1 - h) * NT])
    ca, sa = _rope_tab(pos, 32, 500000.0)
    ci, si = _rope_tab(pos, 16, 500000.0)
    inv = (1.0 / (10000.0 ** np.linspace(0.0, 1.0, 128, dtype=np.float32))).astype(np.float32)
    ang = pos.astype(np.float32)[:, None] * inv[None, :]
    cr, sr = np.cos(ang).astype(np.float32), np.sin(ang).astype(np.float32)
    s = np.arange(128)
    triT = (s[:, None] <= s[None, :]).astype(np.float32)
    negtri = np.where(s[None, :] <= s[:, None], 0.0, NEG).astype(np.float32)
    cc = np.zeros((128, 4), np.float32)
    cc[:, 0] = 0.0 if h == 1 else NEG
    cc[:, 1] = 0.0 if h == 1 else -30000.0
    cc[:, 2] = 1.0 if h == 1 else 0.0
    lg = np.log(1.0 - 2.0 ** (-5.0 - np.arange(8, dtype=np.float64)))
    rdecT = np.zeros((128, 8, 128), np.float32)
    for hh in range(8):
        dq = s[None, :] - s[:, None]
        rdecT[:, hh, :] = np.where(dq >= 0, np.exp(lg[hh] * dq), 0.0)
    rvec = np.zeros((128, 32), np.float32)
    rvec[:, 0:8] = np.exp(lg[None, :] * (127.0 - s[:, None]))
    rvec[:, 8:16] = np.exp(lg[None, :] * (s[:, None] + 1.0))
    rvec[:, 16:24] = np.exp(lg[None, :] * 128.0)
    cC, sC = _rope_tab(pos, 64, 500000.0)
    p = np.arange(128, dtype=np.int32)[:, None]; ib = np.arange(16, dtype=np.int32)[None, :] * 128
    widx = (h * NT + ib + p).astype(np.int32)
    ridx = ((1 - h) * NT + ib + p).astype(np.int32)
    return {
        "cosC": cC, "sinC": sC, "widx": widx, "ridx": ridx,
        "cosA": np.tile(ca, (1, 16)), "sinA": np.tile(sa, (1, 16)),
        "cosI": np.tile(ci, (1, 16)), "sinI": np.tile(si, (1, 16)),
        "cosR": cr, "sinR": sr, "triT": triT, "negtri": negtri, "cc": cc,
        "rdecT": rdecT.reshape(128, 1024), "rvec": rvec,
    }


def _shard_rows(w, c):
    rs = w.shape[0] // NCORES
    return np.ascontiguousarray(w[c * rs:(c + 1) * rs])


def make_in_maps(inputs):
    x = np.asarray(inputs["x"], dtype=np.float32)
    in_maps = []
    for c in range(NCORES):
        b, h = c // 2, c % 2
        m = {"xp": np.ascontiguousarray(np.concatenate([x[b, h * NT:(h + 1) * NT], x[b, (1 - h) * NT:(2 - h) * NT]], 0))}
        m["w_in"] = _shard_rows(inputs["l0_w_in"], c)
        m["w_out0"] = _shard_rows(inputs["l0_w_out"], c)
        m["f_w1"] = _shard_rows(inputs["l0_ffn_w1"], c)
        m["f_w3"] = _shard_rows(inputs["l0_ffn_w3"], c)
        m["f_w2"] = _shard_rows(inputs["l0_ffn_w2"], c)
        m["gn_g"] = np.asarray(inputs["l0_ret_gn_g"], np.float32).reshape(1, 2048)
        for k in ("l0_ln1_g", "l0_ln1_b", "l0_ln2_g", "l0_ln2_b", "l1_ln1_g", "l1_ln1_b", "l1_ln2_g", "l1_ln2_b"):
            m[k] = np.asarray(inputs[k], np.float32).reshape(1, D)
        m["w_dq"] = _shard_rows(inputs["l1_w_dq_dkv"], c)
        m["w_uq"] = _shard_rows(inputs["l1_w_uq"], c)
        m["w_ukv"] = _shard_rows(inputs["l1_w_ukv"], c)
        m["w_out1"] = _shard_rows(inputs["l1_w_out"], c)
        m["m_w1a"] = np.ascontiguousarray(inputs["l1_moe_w1"][c][:, :2560])
        m["m_w1b"] = np.ascontiguousarray(inputs["l1_moe_w1"][c][:, 2560:])
        m["m_w3a"] = np.ascontiguousarray(inputs["l1_moe_w3"][c][:, :2560])
        m["m_w3b"] = np.ascontiguousarray(inputs["l1_moe_w3"][c][:, 2560:])
        m["m_w2a"] = np.ascontiguousarray(inputs["l1_moe_w2"][c][:2560])
        m["m_w2b"] = np.ascontiguousarray(inputs["l1_moe_w2"][c][2560:])
        m["qn_g"] = np.asarray(inputs["l1_q_norm_g"], np.float32).reshape(1, 1024)
        m["kvn_g"] = np.asarray(inputs["l1_kv_norm_g"], np.float32).reshape(1, 512)
        m["routerT"] = np.ascontiguousarray(np.asarray(inputs["l1_router"], np.float32).T)
        m.update(_core_constants(c))
        in_maps.append(m)
    return in_maps


def kernel(**inputs):
    nc = build_program()
    in_maps = make_in_maps(inputs)
    res = run_bass_kernel_spmd(nc, in_maps, core_ids=list(range(NCORES)))
    x = np.asarray(inputs["x"])
    out = np.empty(x.shape, np.float32)
    for c in range(NCORES):
        b, h = c // 2, c % 2
        out[b, h * NT:(h + 1) * NT] = res.results[c]["out"]
    return out
```
